# Optimizing a Trainium2 kernel written in Bass

```python
import jax, jax.numpy as jnp
from jax import lax
import numpy as np

D_MODEL = 1024
BATCH = 2
SEQ = 8192
DEPTH = 1

FOURIER_GROUPS = 4
FOURIER_GROUP_DIM = 128
D_FOURIER = FOURIER_GROUPS * FOURIER_GROUP_DIM
D_CONV = D_MODEL
CONV_WIDTH = 3
N_BRANCHES = 2
D_IN_PROJ = D_FOURIER + 3 * D_CONV + N_BRANCHES * D_MODEL
N_EXPERTS = 16
EC_CAPACITY = 2
D_EXPERT = 2048
N_MOD = 6
RMS_EPS = 1e-6

kernel_name = "hybrid_fourier_shortconv_ecmoe_block"


def rms_norm(x, g):
    xf = x.astype(jnp.float32)
    y = xf * lax.rsqrt(jnp.mean(xf * xf, axis=-1, keepdims=True) + RMS_EPS)
    return (y * g.astype(jnp.float32)).astype(x.dtype)


def modulate(h, shift, scale):
    return h * (1 + scale[:, None, :]) + shift[:, None, :]


def fourier_mix(u):
    b, s, _ = u.shape
    ug = u.reshape(b, s, FOURIER_GROUPS, FOURIER_GROUP_DIM).astype(jnp.float32)
    f = jnp.fft.fftn(ug, axes=(1, 3), norm="ortho")
    return jnp.real(f).reshape(b, s, D_FOURIER).astype(u.dtype)


def centred_conv3(u, w):
    s = u.shape[1]
    up = jnp.pad(u, ((0, 0), (1, 1), (0, 0)))
    return up[:, 0:s] * w[0] + up[:, 1:s + 1] * w[1] + up[:, 2:s + 2] * w[2]


def expert_choice_moe(h, w_router, b_router, w_gate_e, w_up_e, w_down_e):
    b, s, d = h.shape
    cap = EC_CAPACITY * s // N_EXPERTS
    logits = jnp.einsum('bsd,de->bse', h, w_router).astype(jnp.float32) + b_router.astype(jnp.float32)
    probs = jax.nn.softmax(logits, axis=-1)
    vals, idx = lax.top_k(jnp.swapaxes(probs, 1, 2), cap)
    xe = jax.vmap(lambda hb, ib: hb[ib])(h, idx)
    a = jnp.einsum('becd,edf->becf', xe, w_gate_e)
    u = jnp.einsum('becd,edf->becf', xe, w_up_e)
    y = jnp.einsum('becf,efd->becd', jax.nn.silu(a) * u, w_down_e)
    y = y * vals[..., None].astype(y.dtype)
    return jax.vmap(lambda yb, ib: jnp.zeros((s, d), yb.dtype).at[ib.reshape(-1)].add(yb.reshape(-1, d)))(y, idx)


def setup_inputs(seed: int = 0) -> dict:
    key = jax.random.key(seed)
    ks = jax.random.split(key, 20)
    f32 = jnp.float32
    nrm = lambda k, shape, scale: (jax.random.normal(k, shape, f32) * scale).astype(f32)
    D = D_MODEL
    return {
        "x": nrm(ks[0], (BATCH, SEQ, D), 1.0),
        "c": nrm(ks[1], (BATCH, D), 1.0),
        "w_ada": nrm(ks[2], (DEPTH, D, N_MOD * D), 0.5 * D ** -0.5),
        "b_ada": nrm(ks[3], (DEPTH, N_MOD * D), 0.02),
        "g_norm_mix": 1.0 + nrm(ks[4], (DEPTH, D), 0.02),
        "w_in": nrm(ks[5], (DEPTH, D, D_IN_PROJ), D ** -0.5),
        "b_gate": nrm(ks[6], (DEPTH, N_BRANCHES * D), 0.02),
        "w_fourier": nrm(ks[7], (DEPTH, D_FOURIER, D), D_FOURIER ** -0.5),
        "w_conv": nrm(ks[8], (DEPTH, CONV_WIDTH, D_CONV), CONV_WIDTH ** -0.5),
        "w_conv_out": nrm(ks[9], (DEPTH, D_CONV, D), D_CONV ** -0.5),
        "w_o": nrm(ks[10], (DEPTH, D, D), D ** -0.5),
        "g_norm_moe": 1.0 + nrm(ks[11], (DEPTH, D), 0.02),
        "w_router": nrm(ks[12], (DEPTH, D, N_EXPERTS), D ** -0.5),
        "b_router": nrm(ks[13], (DEPTH, N_EXPERTS), 0.01),
        "w_gate_e": nrm(ks[14], (DEPTH, N_EXPERTS, D, D_EXPERT), D ** -0.5),
        "w_up_e": nrm(ks[15], (DEPTH, N_EXPERTS, D, D_EXPERT), D ** -0.5),
        "w_down_e": nrm(ks[16], (DEPTH, N_EXPERTS, D_EXPERT, D), D_EXPERT ** -0.5),
        "g_final": 1.0 + nrm(ks[17], (D,), 0.02),
    }


def reference(x, c, w_ada, b_ada, g_norm_mix, w_in, b_gate, w_fourier, w_conv, w_conv_out, w_o,
              g_norm_moe, w_router, b_router, w_gate_e, w_up_e, w_down_e, g_final):
    D = D_MODEL
    c_act = jax.nn.silu(c)
    for l in range(DEPTH):
        mod = jnp.einsum('bd,dm->bm', c_act, w_ada[l]) + b_ada[l]
        shift_m, scale_m, gate_m, shift_f, scale_f, gate_f = jnp.split(mod, N_MOD, axis=-1)

        h = modulate(rms_norm(x, g_norm_mix[l]), shift_m, scale_m)
        p = jnp.einsum('bsd,dk->bsk', h, w_in[l])
        o1 = D_FOURIER
        o2 = o1 + D_CONV
        o3 = o2 + D_CONV
        o4 = o3 + D_CONV
        u_f = p[..., :o1]
        v, b_g, c_g = p[..., o1:o2], p[..., o2:o3], p[..., o3:o4]
        g_logits = p[..., o4:] + b_gate[l]
        gate_a = jax.nn.sigmoid(g_logits[..., :D])
        gate_b = jax.nn.sigmoid(g_logits[..., D:])
        y_a = jnp.einsum('bsk,kd->bsd', fourier_mix(u_f), w_fourier[l])
        y_b = jnp.einsum('bsk,kd->bsd', b_g * centred_conv3(c_g * v, w_conv[l]), w_conv_out[l])
        z = gate_a * y_a + gate_b * y_b
        mix_out = jnp.einsum('bsd,de->bse', z, w_o[l])
        x = x + gate_m[:, None, :] * mix_out

        h2 = modulate(rms_norm(x, g_norm_moe[l]), shift_f, scale_f)
        moe_out = expert_choice_moe(h2, w_router[l], b_router[l], w_gate_e[l], w_up_e[l], w_down_e[l])
        x = x + gate_f[:, None, :] * moe_out
    return rms_norm(x, g_final)
```

```python
import numpy as np
import ml_dtypes
from contextlib import ExitStack
import concourse.bass as bass
import concourse.mybir as mybir
from concourse.bass_utils import run_bass_kernel_spmd

F32 = mybir.dt.float32
BF16 = mybir.dt.bfloat16
I32 = mybir.dt.int32
ALU = mybir.AluOpType
AF = mybir.ActivationFunctionType
AX = mybir.AxisListType
NPBF = ml_dtypes.bfloat16


class Sched:
    ENG = ("pe", "act", "dve", "pool", "sp")
    NDMA = 6

    def __init__(self, nc, es):
        self.nc = nc
        self.ops = {e: [] for e in self.ENG}
        self.cnt = {e: 0 for e in self.ENG}
        self.prog = {e: es.enter_context(nc.semaphore(f"prog_{e}")) for e in ("pe", "act", "dve", "pool")}
        self.dsem = {q: [es.enter_context(nc.semaphore(f"d_{q}{i}")) for i in range(self.NDMA)]
                     for q in ("sp", "act", "pool")}
        self.duse = {q: [0] * self.NDMA for q in ("sp", "act", "pool")}
        self.dnext = {q: 0 for q in ("sp", "act", "pool")}
        self.ccsem = []
        self.es = es
        self.lastw = {}
        self.readers = {}
        self.pregs = {}

    def breg(self, value):
        self.pregs.setdefault(value, None)
        return value

    def _deps(self, reads, writes):
        deps = set()
        for k in reads:
            if k in self.lastw:
                deps.add(self.lastw[k])
        for k in writes:
            if k in self.lastw:
                deps.add(self.lastw[k])
            for t in self.readers.get(k, ()):
                deps.add(t)
        return deps

    def _commit(self, tok, reads, writes):
        for k in writes:
            self.lastw[k] = tok
            self.readers[k] = []
        for k in reads:
            if k not in writes:
                self.readers.setdefault(k, []).append(tok)

    def compute(self, eng, fn, reads=(), writes=()):
        deps = self._deps(reads, writes)
        self.cnt[eng] += 1
        tok = ("c", eng, self.cnt[eng])
        self.ops[eng].append((fn, deps, tok))
        self._commit(tok, reads, writes)
        return tok

    def dma(self, q, fn, reads=(), writes=()):
        deps = self._deps(reads, writes)
        i = self.dnext[q]
        self.dnext[q] = (i + 1) % self.NDMA
        self.duse[q][i] += 1
        n = self.duse[q][i]
        if n > 1:
            deps.add(("d", q, i, n - 1))
        tok = ("d", q, i, n)
        self.ops[q].append((fn, deps, tok))
        self._commit(tok, reads, writes)
        return tok

    def cc(self, fn, reads=(), writes=()):
        deps = self._deps(reads, writes)
        sem = self.es.enter_context(self.nc.semaphore(f"cc{len(self.ccsem)}"))
        self.ccsem.append(sem)
        tok = ("k", len(self.ccsem) - 1)
        self.ops["pool"].append((fn, deps, tok))
        self._commit(tok, reads, writes)
        return tok

    def _semval(self, tok):
        if tok[0] == "c":
            return self.prog[tok[1]], tok[2], f"prog_{tok[1]}"
        if tok[0] == "d":
            return self.dsem[tok[1]][tok[2]], 16 * tok[3], f"d_{tok[1]}{tok[2]}"
        return self.ccsem[tok[1]], 1, f"cc{tok[1]}"

    def emit(self):
        nc = self.nc
        with nc.Block() as block:
            decos = {"pe": block.tensor, "act": block.scalar, "dve": block.vector,
                     "pool": block.gpsimd, "sp": block.sync}
            for ename in self.ENG:
                def body(eng, ename=ename):
                    seen = {}
                    if ename == "pool":
                        for v in list(self.pregs):
                            r = eng.alloc_register(f"bc{v}")
                            eng.reg_mov(r, v)
                            self.pregs[v] = r
                    for fn, deps, tok in self.ops[ename]:
                        need = {}
                        for d in deps:
                            sem, val, key = self._semval(d)
                            if seen.get(key, 0) >= val:
                                continue
                            if key not in need or need[key][1] < val:
                                need[key] = (sem, val)
                        for key, (sem, val) in need.items():
                            eng.wait_ge(sem, val)
                            seen[key] = val
                        ins = fn(eng)
                        sem, _, _ = self._semval(tok)
                        if tok[0] == "c":
                            ins.then_inc(sem, 1)
                        elif tok[0] == "d":
                            ins.then_inc(sem, 16)
                        else:
                            ins.then_inc(sem)
                    if ename in ("sp", "act", "pool"):
                        for i in range(self.NDMA):
                            n = self.duse[ename][i]
                            if n > 0 and seen.get(f"d_{ename}{i}", 0) < 16 * n:
                                eng.wait_ge(self.dsem[ename][i], 16 * n)
                    if ename == "pool":
                        for j, sem in enumerate(self.ccsem):
                            if seen.get(f"cc{j}", 0) < 1:
                                eng.wait_ge(sem, 1)
                decos[ename](body)


def _sb(nc, es, name, shape, dt):
    return es.enter_context(nc.sbuf_tensor(name, shape, dt))


def _ps(nc, es, name, shape, dt):
    return es.enter_context(nc.psum_tensor(name, shape, dt))


NT = 2048
NTILE = NT // 128
D = 1024
KC = 8
EPS = 1e-6


def emit_mod(nc, es, S, cT, w_ada, b_ada, mod_row, ncols=6144, col0=0):
    cT_sb = _sb(nc, es, "cT_sb", [128, KC], F32)
    cact = _sb(nc, es, "cact", [128, KC], BF16)
    bada = _sb(nc, es, "bada", [1, 6144], F32)
    wada = [_sb(nc, es, f"wada{i}", [128, KC, 512], BF16) for i in range(2)]
    ps_mod = _ps(nc, es, "ps_mod", [128, 512], F32)
    S.dma("sp", lambda e: e.dma_start(out=cT_sb[:], in_=cT), writes=["cT_sb"])
    S.dma("sp", lambda e: e.dma_start(out=bada[:], in_=b_ada), writes=["bada"])
    S.compute("act", lambda e: e.activation(out=cact[:], in_=cT_sb[:], func=AF.Silu),
              reads=["cT_sb"], writes=["cact"])
    wv = w_ada.rearrange("(kc p) n -> p kc n", p=128)
    for nb in range(12):
        buf = wada[nb % 2]
        S.dma("pool", lambda e, nb=nb, buf=buf: e.dma_start(out=buf[:], in_=wv[:, :, nb * 512:(nb + 1) * 512]),
              writes=[f"wada{nb % 2}"])

        def mm(e, buf=buf):
            ins = None
            for kc in range(KC):
                ins = e.matmul(ps_mod[0:1, :], lhsT=cact[:, kc:kc + 1], rhs=buf[:, kc, :],
                               start=(kc == 0), stop=(kc == KC - 1))
            return ins
        S.compute("pe", mm, reads=["cact", f"wada{nb % 2}"], writes=["ps_mod"])
        S.compute("dve", lambda e, nb=nb: e.tensor_tensor(out=mod_row[0:1, nb * 512:(nb + 1) * 512],
                                                          in0=ps_mod[0:1, :], in1=bada[0:1, nb * 512:(nb + 1) * 512],
                                                          op=ALU.add),
                  reads=["ps_mod", "bada"], writes=["mod_row"])


def emit_bcast(nc, S, ones_row, row_ap, row_key, ps_bc, out_tile, out_key):
    for half in range(2):
        S.compute("pe", lambda e, half=half: e.matmul(ps_bc[:, :], lhsT=ones_row[0:1, :],
                                                      rhs=row_ap[0:1, half * 512:(half + 1) * 512],
                                                      start=True, stop=True),
                  reads=[row_key, "ones_row"], writes=["ps_bc"])
        S.compute("act", lambda e, half=half: e.copy(out=out_tile[:, half * 512:(half + 1) * 512], in_=ps_bc[:, :]),
                  reads=["ps_bc"], writes=[out_key])


def emit_norm_tile(nc, S, x_tile, x_key, ss_col, rstd_col, junk, a_bc, a_key, s_bc, s_key, t32, h_bf, h_key, tag):
    jk = junk[:] if junk is not None else h_bf
    jkey = "junk" if junk is not None else h_key
    S.compute("act", lambda e: e.activation(out=jk, in_=x_tile, func=AF.Square, accum_out=ss_col),
              reads=[x_key], writes=[jkey, "ss" + tag])
    S.compute("act", lambda e: e.activation(out=rstd_col, in_=ss_col, func=AF.Sqrt, scale=1.0 / D, bias=EPS),
              reads=["ss" + tag], writes=["rstd" + tag])
    S.compute("dve", lambda e: e.reciprocal(out=rstd_col, in_=rstd_col),
              reads=["rstd" + tag], writes=["rstd" + tag])
    S.compute("dve", lambda e: e.scalar_tensor_tensor(out=t32[:], in0=x_tile, scalar=rstd_col, in1=a_bc[:],
                                                      op0=ALU.mult, op1=ALU.mult),
              reads=[x_key, "rstd" + tag, a_key], writes=["t32"])
    S.compute("pool", lambda e: e.tensor_tensor(out=h_bf, in0=t32[:], in1=s_bc[:], op=ALU.add),
              reads=["t32", s_key], writes=[h_key])


def build_l1():
    nc = bass.Bass("TRN2", target_bir_lowering=False)
    x = nc.dram_tensor("x", [NT, D], F32, kind="ExternalInput").ap()
    cT = nc.dram_tensor("cT", [128, KC], F32, kind="ExternalInput").ap()
    w_ada = nc.dram_tensor("w_ada", [D, 6144], F32, kind="ExternalInput").ap()
    b_ada = nc.dram_tensor("b_ada", [1, 6144], F32, kind="ExternalInput").ap()
    g_mix = nc.dram_tensor("g_mix", [1, D], F32, kind="ExternalInput").ap()
    w_inf = nc.dram_tensor("w_inf", [D, 512], F32, kind="ExternalInput").ap()
    ident_in = nc.dram_tensor("ident", [128, 128], BF16, kind="ExternalInput").ap()
    u_f = nc.dram_tensor("u_f", [NT, 512], BF16, kind="ExternalOutput").ap()
    mod_out = nc.dram_tensor("mod_out", [1, 6144], F32, kind="ExternalOutput").ap()
    with ExitStack() as es:
        S = Sched(nc, es)
        ident = _sb(nc, es, "ident_sb", [128, 128], BF16)
        ones_row = _sb(nc, es, "ones_row", [1, 128], F32)
        mod_row = _sb(nc, es, "mod_row", [1, 6144], F32)
        g_row = _sb(nc, es, "g_row", [1, D], F32)
        a1_row = _sb(nc, es, "a1_row", [1, D], F32)
        a1_bc = _sb(nc, es, "a1_bc", [128, D], F32)
        s1_bc = _sb(nc, es, "s1_bc", [128, D], F32)
        winf = _sb(nc, es, "winf", [128, KC, 512], BF16)
        xt = [_sb(nc, es, f"xt{i}", [128, D], F32) for i in range(2)]
        junk = _sb(nc, es, "junk", [128, D], BF16)
        t32 = _sb(nc, es, "t32", [128, D], F32)
        hbf = [_sb(nc, es, f"hbf{i}", [128, D], BF16) for i in range(2)]
        ss = _sb(nc, es, "ss", [128, NTILE], F32)
        rstd = _sb(nc, es, "rstd", [128, NTILE], F32)
        hT = _sb(nc, es, "hT", [128, KC, NT], BF16)
        ubf = [_sb(nc, es, f"ubf{i}", [128, 512], BF16) for i in range(2)]
        ps_bc = _ps(nc, es, "ps_bc", [128, 512], F32)
        ps_tr = [_ps(nc, es, f"ps_tr{i}", [128, D], BF16) for i in range(2)]
        ps_u = [_ps(nc, es, f"ps_u{i}", [128, 512], F32) for i in range(2)]

        S.dma("sp", lambda e: e.dma_start(out=ident[:], in_=ident_in), writes=["ident"])
        S.dma("sp", lambda e: e.dma_start(out=g_row[:], in_=g_mix), writes=["g_row"])
        S.compute("dve", lambda e: e.memset(ones_row[:], 1.0), writes=["ones_row"])
        emit_mod(nc, es, S, cT, w_ada, b_ada, mod_row)
        S.dma("sp", lambda e: e.dma_start(out=mod_out, in_=mod_row[:]), reads=["mod_row"])
        S.dma("pool", lambda e: e.dma_start(out=winf[:], in_=w_inf.rearrange("(kc p) n -> p kc n", p=128)),
              writes=["winf"])
        S.compute("dve", lambda e: e.scalar_tensor_tensor(out=a1_row[:], in0=mod_row[0:1, 1024:2048], scalar=1.0,
                                                          in1=g_row[:], op0=ALU.add, op1=ALU.mult),
                  reads=["mod_row", "g_row"], writes=["a1_row"])
        emit_bcast(nc, S, ones_row, a1_row, "a1_row", ps_bc, a1_bc, "a1_bc")
        emit_bcast(nc, S, ones_row, mod_row[0:1, 0:1024], "mod_row", ps_bc, s1_bc, "s1_bc")
        for i in range(NTILE):
            xb = xt[i % 2]
            S.dma("sp", lambda e, i=i, xb=xb: e.dma_start(out=xb[:], in_=x[i * 128:(i + 1) * 128, :]),
                  writes=[f"xt{i % 2}"])
            hb = hbf[i % 2]
            emit_norm_tile(nc, S, xb[:], f"xt{i % 2}", ss[:, i:i + 1], rstd[:, i:i + 1], junk, a1_bc, "a1_bc",
                           s1_bc, "s1_bc", t32, hb[:], f"hbf{i % 2}", f"{i}")
            pt = ps_tr[i % 2]

            def tr(e, hb=hb, pt=pt):
                ins = None
                for kc in range(KC):
                    ins = e.transpose(out=pt[:, kc * 128:(kc + 1) * 128], in_=hb[:, kc * 128:(kc + 1) * 128],
                                      identity=ident[:])
                return ins
            S.compute("pe", tr, reads=[f"hbf{i % 2}", "ident"], writes=[f"ps_tr{i % 2}"])
            S.compute("act", lambda e, i=i, pt=pt: e.copy(out=hT[:, :, i * 128:(i + 1) * 128],
                                                          in_=pt[:, :].rearrange("p (k t) -> p k t", k=KC)),
                      reads=[f"ps_tr{i % 2}"], writes=[f"hT{i}"])
            pu = ps_u[i % 2]

            def mm(e, i=i, pu=pu):
                ins = None
                for kc in range(KC):
                    ins = e.matmul(pu[:, :], lhsT=hT[:, kc, i * 128:(i + 1) * 128], rhs=winf[:, kc, :],
                                   start=(kc == 0), stop=(kc == KC - 1))
                return ins
            S.compute("pe", mm, reads=[f"hT{i}", "winf"], writes=[f"ps_u{i % 2}"])
            ub = ubf[i % 2]
            S.compute("dve", lambda e, ub=ub, pu=pu: e.tensor_copy(out=ub[:], in_=pu[:, :]),
                      reads=[f"ps_u{i % 2}"], writes=[f"ubf{i % 2}"])
            S.dma("sp", lambda e, i=i, ub=ub: e.dma_start(out=u_f[i * 128:(i + 1) * 128, :], in_=ub[:]),
                  reads=[f"ubf{i % 2}"])
        S.emit()
    return nc


def dft_tables():
    a = np.arange(64)[:, None]
    c = np.arange(64)[None, :]
    th = 2 * np.pi * a * c / 64.0
    f1 = np.concatenate([np.cos(th), -np.sin(th)], axis=1)
    bp = np.arange(128)[:, None, None]
    cc = np.arange(64)[None, :, None]
    dd = np.arange(128)[None, None, :]
    th2 = 2 * np.pi * bp * (cc + 64 * dd) / 8192.0
    stab = np.concatenate([np.sin(th2), np.cos(th2), -np.sin(th2)], axis=2)
    ch = np.arange(128)[:, None]
    l = np.arange(128)[None, :]
    th3 = 2 * np.pi * ch * l / 128.0
    cs = np.concatenate([np.cos(th3), np.sin(th3)], axis=1) / 1024.0
    return f1.astype(NPBF), stab.astype(NPBF), cs.astype(NPBF)


def emit_dft(nc, es, S, U_dram, U_key, f1_in, stab_in, cs_in, A_out, stop_after=3):
    R1 = _sb(nc, es, "dftR1", [128, 16384], BF16)
    R2 = _sb(nc, es, "dftR2", [128, 16384], BF16)
    f1 = _sb(nc, es, "dft_f1", [64, 128], BF16)
    cs = _sb(nc, es, "dft_cs", [128, 256], BF16)
    stb = [_sb(nc, es, f"dft_stb{i}", [128, 8, 384], BF16) for i in range(2)]
    ps1 = [_ps(nc, es, f"dft_ps1_{i}", [128, 512], F32) for i in range(2)]
    ps2 = [_ps(nc, es, f"dft_ps2_{i}", [128, 512], F32) for i in range(2)]
    ps3 = [_ps(nc, es, f"dft_ps3_{i}", [128, 512], F32) for i in range(2)]
    S.dma("sp", lambda e: e.dma_start(out=f1[:], in_=f1_in), writes=["dft_f1"])
    S.dma("sp", lambda e: e.dma_start(out=cs[:], in_=cs_in), writes=["dft_cs"])
    S.dma("sp", lambda e: e.dma_start(out=R1[0:64, :], in_=U_dram.rearrange("(a b) c -> a (b c)", a=64)),
          reads=[U_key], writes=["dftU"])
    Uv = R1[0:64, :].rearrange("p (b c) -> p c b", c=128)
    Yv = R2[:, :].rearrange("p (ch r) -> p r ch", r=128)
    Zv = R1[:, :].rearrange("p (r d c) -> p r d c", r=2, d=128, c=64)
    for blk in range(32):
        p1 = ps1[blk % 2]

        def mm(e, blk=blk, p1=p1):
            ins = None
            for q in range(4):
                ins = e.matmul(p1[:, q * 128:(q + 1) * 128], lhsT=Uv[:, blk * 4 + q, :], rhs=f1[:, :],
                               start=True, stop=True)
            return ins
        S.compute("pe", mm, reads=["dftU", "dft_f1"], writes=[f"dft_ps1_{blk % 2}"])
        eng = "act" if blk % 2 == 0 else "dve"
        if eng == "act":
            S.compute("act", lambda e, blk=blk, p1=p1: e.copy(out=R2[:, blk * 512:(blk + 1) * 512], in_=p1[:, :]),
                      reads=[f"dft_ps1_{blk % 2}"], writes=[f"dftY{blk}"])
        else:
            S.compute("dve", lambda e, blk=blk, p1=p1: e.tensor_copy(out=R2[:, blk * 512:(blk + 1) * 512], in_=p1[:, :]),
                      reads=[f"dft_ps1_{blk % 2}"], writes=[f"dftY{blk}"])
    allY = [f"dftY{blk}" for blk in range(32)]
    if stop_after == 1:
        S.dma("sp", lambda e: e.dma_start(out=A_out[:, :], in_=R2[:, 0:8192]), reads=allY)
        return
    import os
    NC2 = int(os.environ.get('NC2', '64'))
    for c in range(NC2):
        if c % 8 == 0:
            sb_ = stb[(c // 8) % 2]
            S.dma("sp", lambda e, c=c, sb_=sb_: e.dma_start(out=sb_[:], in_=stab_in[:, c:c + 8, :]),
                  writes=[f"dft_stb{(c // 8) % 2}"])
        sb_ = stb[(c // 8) % 2]
        p2 = ps2[c % 2]
        off = 0

        def mm2(e, c=c, sb_=sb_, p2=p2, off=off):
            e.matmul(p2[:, off:off + 256], lhsT=Yv[:, c, :], rhs=sb_[:, c % 8, 128:384], start=True, stop=False)
            return e.matmul(p2[:, off:off + 256], lhsT=Yv[:, 64 + c, :], rhs=sb_[:, c % 8, 0:256],
                            start=False, stop=True)
        S.compute("pe", mm2, reads=allY + [f"dft_stb{(c // 8) % 2}"], writes=[f"dft_ps2_{c % 2}"])
        src = p2[:, off:off + 256].rearrange("p (r d) -> p r d", r=2)
        wr = [f"dftZ{c}"] + (["dftU"] if c == 0 else [])
        if c % 2 == 0 or os.environ.get("ALLACT"):
            S.compute("act", lambda e, c=c, src=src, p2=p2, off=off: e.copy(out=Zv[:, :, :, c], in_=src),
                      reads=[f"dft_ps2_{c % 2}"], writes=wr)
        else:
            S.compute("dve", lambda e, c=c, src=src, p2=p2, off=off: e.tensor_copy(out=Zv[:, :, :, c], in_=src),
                      reads=[f"dft_ps2_{c % 2}"], writes=wr)
    allZ = [f"dftZ{c}" for c in range(NC2)]
    if stop_after == 2:
        S.dma("sp", lambda e: e.dma_start(out=A_out[:, :], in_=R1[:, 0:8192]), reads=allZ)
        return
    for kb in range(16):
        p3 = ps3[kb % 2]

        def mm3(e, kb=kb, p3=p3):
            e.matmul(p3[:, :], lhsT=cs[:, 0:128], rhs=R1[:, kb * 512:(kb + 1) * 512], start=True, stop=False)
            return e.matmul(p3[:, :], lhsT=cs[:, 128:256], rhs=R1[:, 8192 + kb * 512:8192 + (kb + 1) * 512],
                            start=False, stop=True)
        S.compute("pe", mm3, reads=allZ + ["dft_cs"], writes=[f"dft_ps3_{kb % 2}"])
        wr = [f"dftA{kb}"] + (allY if kb == 0 else [])
        if kb % 2 == 0:
            S.compute("act", lambda e, kb=kb, p3=p3: e.copy(out=R2[:, kb * 512:(kb + 1) * 512], in_=p3[:, :]),
                      reads=[f"dft_ps3_{kb % 2}"], writes=wr)
        else:
            S.compute("dve", lambda e, kb=kb, p3=p3: e.tensor_copy(out=R2[:, kb * 512:(kb + 1) * 512], in_=p3[:, :]),
                      reads=[f"dft_ps3_{kb % 2}"], writes=wr)
        if kb % 4 == 3:
            j = kb // 4
            S.dma("sp", lambda e, j=j: e.dma_start(out=A_out[:, j * 2048:(j + 1) * 2048],
                                                   in_=R2[:, j * 2048:(j + 1) * 2048]),
                  reads=[f"dftA{k}" for k in range(4 * j, 4 * j + 4)], writes=[f"A_out{j}"])


def build_l2(stop_after=3):
    nc = bass.Bass("TRN2", target_bir_lowering=False)
    U = nc.dram_tensor("U", [8192, 128], BF16, kind="ExternalInput").ap()
    f1_in = nc.dram_tensor("f1", [64, 128], BF16, kind="ExternalInput").ap()
    stab_in = nc.dram_tensor("stab", [128, 64, 384], BF16, kind="ExternalInput").ap()
    cs_in = nc.dram_tensor("cs", [128, 256], BF16, kind="ExternalInput").ap()
    A_out = nc.dram_tensor("A_out", [128, 8192], BF16, kind="ExternalOutput").ap()
    with ExitStack() as es:
        S = Sched(nc, es)
        emit_dft(nc, es, S, U, "U_in", f1_in, stab_in, cs_in, A_out, stop_after=stop_after)
        S.emit()
    return nc


HTW = 2176


def emit_norm1_all(nc, es, S, x, xh, ident, a_bc, s_bc, xt, junk, t32, hbf, ss, rstd, hT, ps_tr):
    for i in range(NTILE + 1):
        xb = xt[i % 2]
        src = x[i * 128:(i + 1) * 128, :] if i < NTILE else xh
        S.dma("sp", lambda e, xb=xb, src=src: e.dma_start(out=xb[:], in_=src), writes=[f"xt{i % 2}"])
        hb = hbf[i % 2]
        emit_norm_tile(nc, S, xb[:], f"xt{i % 2}", ss[:, i:i + 1], rstd[:, i:i + 1], None, a_bc, "bcA",
                       s_bc, "bcS", t32, hb[:], f"hbf{i % 2}", f"n1_{i}")
        pt = ps_tr

        def tr(e, hb=hb, pt=pt):
            ins = None
            for kc in range(KC):
                ins = e.transpose(out=pt[:, kc * 128:(kc + 1) * 128], in_=hb[:, kc * 128:(kc + 1) * 128],
                                  identity=ident[:])
            return ins
        S.compute("pe", tr, reads=[f"hbf{i % 2}", "ident"], writes=["ps_tr"])
        S.compute("act", lambda e, i=i, pt=pt: e.copy(out=hT[:, :, i * 128:(i + 1) * 128],
                                                      in_=pt[:, :].rearrange("p (k t) -> p k t", k=KC)),
                  reads=["ps_tr"], writes=[f"hT{i}"])


def build_l3():
    nc = bass.Bass("TRN2", target_bir_lowering=False)
    di = lambda name, shape, dt: nc.dram_tensor(name, shape, dt, kind="ExternalInput").ap()
    do = lambda name, shape, dt: nc.dram_tensor(name, shape, dt, kind="ExternalOutput").ap()
    x = di("x", [NT, D], F32)
    xh = di("xh", [128, D], F32)
    hmask_in = di("hmask", [128, 2], F32)
    mod_in = di("mod", [1, 6144], F32)
    g_mix = di("g_mix", [1, D], F32)
    g_moe = di("g_moe", [1, D], F32)
    w_in = di("w_in", [D, 5632], F32)
    bgate_in = di("bgate", [128, 16], F32)
    wconv_in = di("wconv", [128, 8, 3], F32)
    w_co = di("w_co", [D, D], F32)
    w_fo = di("w_fo", [512, D], F32)
    w_o = di("w_o", [D, D], F32)
    w_r = di("w_r", [D, 16], F32)
    b_r = di("b_r", [1, 16], F32)
    AT_in = di("AT", [128, 4, NT], BF16)
    ident_in = di("ident", [128, 128], BF16)
    identf_in = di("identf", [128, 128], F32)
    xmid_out = do("x_mid", [NT, D], F32)
    h2_out = do("h2", [NT, D], BF16)
    probsT_out = do("probsT", [16, NT], F32)
    with ExitStack() as es:
        S = Sched(nc, es)
        sb = lambda name, shape, dt: _sb(nc, es, name, shape, dt)
        ident = sb("ident_sb", [128, 128], BF16)
        identf = sb("identf_sb", [128, 128], F32)
        br_bc = sb("br_bc", [128, 16], F32)
        hmask = sb("hmask_sb", [128, 2], F32)
        bgate = sb("bgate_sb", [128, 16], F32)
        wconv = sb("wconv_sb", [128, 8, 3], F32)
        bcA = sb("bcA", [128, D], F32)
        bcS = sb("bcS", [128, D], F32)
        bcG = sb("bcG", [128, D], F32)
        xt = [sb(f"xt{i}", [128, D], F32) for i in range(2)]
        t32 = sb("t32", [128, D], F32)
        hbf = [sb(f"hbf{i}", [128, D], BF16) for i in range(2)]
        h2T = [sb(f"h2T{i}", [128, KC, 128], BF16) for i in range(2)]
        ss = sb("ss", [128, 2 * (NTILE + 1)], F32)
        rstd = sb("rstd", [128, 2 * (NTILE + 1)], F32)
        sm = sb("sm", [128, 8 * NTILE], F32)
        prT = [sb(f"prT{i}", [16, 128], F32) for i in range(2)]
        lg = [sb(f"lg{i}", [128, 16], F32) for i in range(2)]
        ex = [sb(f"ex{i}", [128, 16], F32) for i in range(2)]
        hT = sb("hT", [128, KC, HTW], BF16)
        cvb = [sb(f"cvb{i}", [128, 2050], BF16) for i in range(2)]
        sc = [sb(f"sc{i}", [128, 512], F32) for i in range(4)]
        convT = sb("convT", [128, KC, NT], BF16)
        zT = sb("zT", [128, KC, NT], BF16)
        wbuf = [sb(f"wbuf{i}", [128, KC, 512], BF16) for i in range(3)]
        wbig = sb("wbig", [128, KC, D], BF16)
        wr = sb("wr", [128, KC, 16], BF16)
        psA = [_ps(nc, es, f"psA{i}", [128, 512], F32) for i in range(2)]
        psB = [_ps(nc, es, f"psB{i}", [128, 512], F32) for i in range(2)]
        psM = [_ps(nc, es, f"psM{i}", [128, 512], F32) for i in range(2)]
        ps_tr = _ps(nc, es, "ps_tr", [128, D], BF16)

        S.dma("sp", lambda e: e.dma_start(out=ident[:], in_=ident_in), writes=["ident"])
        S.dma("sp", lambda e: e.dma_start(out=identf[:], in_=identf_in), writes=["identf"])
        S.dma("sp", lambda e: e.dma_start(out=hmask[:], in_=hmask_in), writes=["hmask"])
        S.dma("sp", lambda e: e.dma_start(out=bgate[:], in_=bgate_in), writes=["bgate"])
        S.dma("sp", lambda e: e.dma_start(out=wconv[:], in_=wconv_in), writes=["wconv"])
        def bc_load(dst, key, src_row):
            S.dma("sp", lambda e: e.dma_start(out=dst[:], in_=src_row.partition_broadcast(128)), writes=[key])

        def make_ab(scale_lo, shift_lo, g_dram):
            bc_load(bcA, "bcA", mod_in[0:1, scale_lo:scale_lo + 1024])
            bc_load(t32, "t32", g_dram[0:1, :])
            bc_load(bcS, "bcS", mod_in[0:1, shift_lo:shift_lo + 1024])
            S.compute("dve", lambda e: e.scalar_tensor_tensor(out=bcA[:], in0=bcA[:], scalar=1.0, in1=t32[:],
                                                              op0=ALU.add, op1=ALU.mult),
                      reads=["bcA", "t32"], writes=["bcA"])
        make_ab(1024, 0, g_mix)
        S.dma("sp", lambda e: e.dma_start(out=br_bc[:], in_=b_r[0:1, :].partition_broadcast(128)), writes=["br_bc"])
        wv = w_in.rearrange("(kc p) n -> p kc n", p=128)
        wctr = [0]

        def load_piece(col0):
            i = wctr[0] % 3
            wctr[0] += 1
            S.dma("pool", lambda e, i=i, col0=col0: e.dma_start(out=wbuf[i][:], in_=wv[:, :, col0:col0 + 512]),
                  writes=[f"wbuf{i}"])
            return i
        S.dma("pool", lambda e: e.dma_start(out=wbig[:], in_=w_co.rearrange("(kc p) n -> p kc n", p=128)),
              writes=["wbig"])
        S.dma("pool", lambda e: e.dma_start(out=wr[:], in_=w_r.rearrange("(kc p) n -> p kc n", p=128)),
              writes=["wr"])
        emit_norm1_all(nc, es, S, x, xh, ident, bcA, bcS, xt, None, t32, hbf, ss, rstd, hT, ps_tr)
        allhT = [f"hT{i}" for i in range(NTILE + 1)]

        def mm8(e, ps, wtile, ncol0, rhs_fn, n):
            ins = None
            for kc in range(KC):
                ins = e.matmul(ps[:, 0:n], lhsT=wtile[:, kc, ncol0:ncol0 + 128], rhs=rhs_fn(kc),
                               start=(kc == 0), stop=(kc == KC - 1))
            return ins
        ctr = [0]
        for qg in range(2):
            iv = load_piece(512 + 512 * qg)
            ic = load_piece(2560 + 512 * qg)
            ib = load_piece(1536 + 512 * qg)
            for qq in range(4):
                q = 4 * qg + qq
                cv = cvb[q % 2]
                cvk = f"cvb{q % 2}"
                for tb in range(5):
                    n = 512 if tb < 4 else 2
                    c0 = tb * 512
                    pa, pb = psA[ctr[0] % 2], psB[ctr[0] % 2]
                    pak, pbk = f"psA{ctr[0] % 2}", f"psB{ctr[0] % 2}"
                    sv = sc[ctr[0] % 2]
                    svk = f"sc{ctr[0] % 2}"
                    ctr[0] += 1
                    S.compute("pe", lambda e, pa=pa, qq=qq, c0=c0, n=n, iv=iv: mm8(
                        e, pa, wbuf[iv], qq * 128, lambda kc: hT[:, kc, c0:c0 + n], n),
                        reads=allhT + [f"wbuf{iv}"], writes=[pak])
                    S.compute("pe", lambda e, pb=pb, qq=qq, c0=c0, n=n, ic=ic: mm8(
                        e, pb, wbuf[ic], qq * 128, lambda kc: hT[:, kc, c0:c0 + n], n),
                        reads=allhT + [f"wbuf{ic}"], writes=[pbk])
                    S.compute("act", lambda e, sv=sv, pa=pa, n=n: e.copy(out=sv[:, 0:n], in_=pa[:, 0:n]),
                              reads=[pak], writes=[svk])
                    if tb < 4:
                        S.compute("dve", lambda e, cv=cv, sv=sv, pb=pb, c0=c0: e.tensor_tensor(
                            out=cv[:, 1 + c0:1 + c0 + 512], in0=sv[:, :], in1=pb[:, :], op=ALU.mult),
                            reads=[svk, pbk], writes=[cvk + f"_{tb}"])
                    else:
                        S.compute("dve", lambda e, sv=sv, pb=pb: e.tensor_tensor(
                            out=sv[:, 2:4], in0=sv[:, 0:2], in1=pb[:, 0:2], op=ALU.mult),
                            reads=[svk, pbk], writes=[svk])
                        S.compute("dve", lambda e, cv=cv, sv=sv: e.tensor_tensor(
                            out=cv[:, 0:2050:2049], in0=sv[:, 2:4], in1=hmask[:, :], op=ALU.mult),
                            reads=[svk, "hmask"], writes=[cvk + "_4"])
                cvall = [cvk + f"_{t}" for t in range(5)]
                for tb in range(4):
                    c0 = tb * 512
                    pa = psA[ctr[0] % 2]
                    pak = f"psA{ctr[0] % 2}"
                    t1, t2 = sc[2], sc[3]
                    ctr[0] += 1
                    S.compute("pe", lambda e, pa=pa, qq=qq, c0=c0, ib=ib: mm8(
                        e, pa, wbuf[ib], qq * 128, lambda kc: hT[:, kc, c0:c0 + 512], 512),
                        reads=allhT + [f"wbuf{ib}"], writes=[pak])
                    S.compute("pool", lambda e, cv=cv, q=q, c0=c0, t1=t1: e.tensor_scalar(
                        out=t1[:, :], in0=cv[:, c0:c0 + 512], scalar1=wconv[:, q, 0:1], scalar2=None, op0=ALU.mult),
                        reads=cvall + ["wconv"], writes=["sc2"])
                    S.compute("dve", lambda e, cv=cv, q=q, c0=c0, t1=t1, t2=t2: e.scalar_tensor_tensor(
                        out=t2[:, :], in0=cv[:, c0 + 1:c0 + 513], scalar=wconv[:, q, 1:2], in1=t1[:, :],
                        op0=ALU.mult, op1=ALU.add),
                        reads=cvall + ["wconv", "sc2"], writes=["sc3"])
                    S.compute("dve", lambda e, cv=cv, q=q, c0=c0, t1=t1, t2=t2: e.scalar_tensor_tensor(
                        out=t1[:, :], in0=cv[:, c0 + 2:c0 + 514], scalar=wconv[:, q, 2:3], in1=t2[:, :],
                        op0=ALU.mult, op1=ALU.add),
                        reads=cvall + ["wconv", "sc3"], writes=["sc2"])
                    S.compute("dve", lambda e, q=q, c0=c0, t1=t1, pa=pa: e.tensor_tensor(
                        out=convT[:, q, c0:c0 + 512], in0=t1[:, :], in1=pa[:, :], op=ALU.mult),
                        reads=["sc2", pak], writes=[f"convT{q}"])
        allconv = [f"convT{q}" for q in range(8)]
        for ng in range(2):
            ig = load_piece(4608 + 512 * ng)
            for nn in range(4):
                n_ = 4 * ng + nn
                for tb in range(4):
                    c0 = tb * 512
                    pa, pb = psA[ctr[0] % 2], psB[ctr[0] % 2]
                    pak, pbk = f"psA{ctr[0] % 2}", f"psB{ctr[0] % 2}"
                    sg = sc[ctr[0] % 2]
                    sgk = f"sc{ctr[0] % 2}"
                    ctr[0] += 1
                    S.compute("pe", lambda e, pa=pa, n_=n_, c0=c0: mm8(
                        e, pa, wbig, n_ * 128, lambda kc: convT[:, kc, c0:c0 + 512], 512),
                        reads=allconv + ["wbig"], writes=[pak])
                    S.compute("pe", lambda e, pb=pb, nn=nn, c0=c0, ig=ig: mm8(
                        e, pb, wbuf[ig], nn * 128, lambda kc: hT[:, kc, c0:c0 + 512], 512),
                        reads=allhT + [f"wbuf{ig}"], writes=[pbk])
                    S.compute("act", lambda e, sg=sg, pb=pb, n_=n_: e.activation(
                        out=sg[:, :], in_=pb[:, :], func=AF.Sigmoid, bias=bgate[:, 8 + n_:9 + n_], scale=1.0),
                        reads=[pbk, "bgate"], writes=[sgk])
                    S.compute("dve", lambda e, sg=sg, pa=pa, n_=n_, c0=c0: e.tensor_tensor(
                        out=zT[:, n_, c0:c0 + 512], in0=sg[:, :], in1=pa[:, :], op=ALU.mult),
                        reads=[sgk, pak], writes=[f"zT{n_}_{tb}"])
        AT = convT
        S.dma("sp", lambda e: e.dma_start(out=AT[:, 0:4, :], in_=AT_in), writes=allconv + ["AT"])
        S.dma("pool", lambda e: e.dma_start(out=wbig[:, 0:4, :], in_=w_fo.rearrange("(g p) n -> p g n", p=128)),
              writes=["wbig"])
        for ng in range(2):
            ig = load_piece(3584 + 512 * ng)
            for nn in range(4):
                n_ = 4 * ng + nn
                for tb in range(4):
                    c0 = tb * 512
                    pa, pb = psA[ctr[0] % 2], psB[ctr[0] % 2]
                    pak, pbk = f"psA{ctr[0] % 2}", f"psB{ctr[0] % 2}"
                    sg = sc[ctr[0] % 2]
                    sgk = f"sc{ctr[0] % 2}"
                    tz = sc[2 + ctr[0] % 2]
                    tzk = f"sc{2 + ctr[0] % 2}"
                    ctr[0] += 1

                    def mmf(e, pa=pa, n_=n_, c0=c0):
                        ins = None
                        for g in range(4):
                            ins = e.matmul(pa[:, :], lhsT=wbig[:, g, n_ * 128:(n_ + 1) * 128],
                                           rhs=AT[:, g, c0:c0 + 512], start=(g == 0), stop=(g == 3))
                        return ins
                    S.compute("pe", mmf, reads=["AT", "wbig"], writes=[pak])
                    S.compute("pe", lambda e, pb=pb, nn=nn, c0=c0, ig=ig: mm8(
                        e, pb, wbuf[ig], nn * 128, lambda kc: hT[:, kc, c0:c0 + 512], 512),
                        reads=allhT + [f"wbuf{ig}"], writes=[pbk])
                    S.compute("act", lambda e, sg=sg, pb=pb, n_=n_: e.activation(
                        out=sg[:, :], in_=pb[:, :], func=AF.Sigmoid, bias=bgate[:, n_:n_ + 1], scale=1.0),
                        reads=[pbk, "bgate"], writes=[sgk])
                    S.compute("dve", lambda e, sg=sg, pa=pa, tz=tz: e.tensor_tensor(
                        out=tz[:, :], in0=sg[:, :], in1=pa[:, :], op=ALU.mult),
                        reads=[sgk, pak], writes=[tzk])
                    S.compute("pool", lambda e, tz=tz, n_=n_, c0=c0: e.tensor_tensor(
                        out=zT[:, n_, c0:c0 + 512], in0=zT[:, n_, c0:c0 + 512], in1=tz[:, :], op=ALU.add),
                        reads=[tzk, f"zT{n_}_{tb}"], writes=[f"zT{n_}_{tb}"])
        allz = [f"zT{n_}_{tb}" for n_ in range(8) for tb in range(4)]
        S.dma("pool", lambda e: e.dma_start(out=wbig[:], in_=w_o.rearrange("(kc p) n -> p kc n", p=128)),
              writes=["wbig"])
        make_ab(4096, 3072, g_moe)
        bc_load(bcG, "bcG", mod_in[0:1, 2048:3072])
        for i in range(NTILE):
            xb = xt[i % 2]
            xbk = f"xt{i % 2}"
            S.dma("sp", lambda e, i=i, xb=xb: e.dma_start(out=xb[:], in_=x[i * 128:(i + 1) * 128, :]), writes=[xbk])
            for half in range(2):
                pm = psM[half]

                def mmo(e, i=i, half=half, pm=pm):
                    ins = None
                    for kc in range(KC):
                        ins = e.matmul(pm[:, :], lhsT=zT[:, kc, i * 128:(i + 1) * 128],
                                       rhs=wbig[:, kc, half * 512:(half + 1) * 512],
                                       start=(kc == 0), stop=(kc == KC - 1))
                    return ins
                S.compute("pe", mmo, reads=allz + ["wbig"], writes=[f"psM{half}"])
                S.compute("dve", lambda e, half=half, pm=pm: e.tensor_tensor(
                    out=t32[:, half * 512:(half + 1) * 512], in0=pm[:, :], in1=bcG[:, half * 512:(half + 1) * 512],
                    op=ALU.mult), reads=[f"psM{half}", "bcG"], writes=["t32"])
            S.compute("pool", lambda e, xb=xb: e.tensor_tensor(out=xb[:, :], in0=t32[:, :], in1=xb[:, :], op=ALU.add),
                      reads=["t32", xbk], writes=[xbk])
            S.dma("sp", lambda e, i=i, xb=xb: e.dma_start(out=xmid_out[i * 128:(i + 1) * 128, :], in_=xb[:, :]), reads=[xbk])
            hb = hbf[i % 2]
            hbk = f"hbf{i % 2}"
            emit_norm_tile(nc, S, xb[:, :], xbk, ss[:, 17 + i:18 + i], rstd[:, 17 + i:18 + i], None, bcA, "bcA",
                           bcS, "bcS", t32, hb[:], hbk, f"n2_{i}")
            S.dma("sp", lambda e, i=i, hb=hb: e.dma_start(out=h2_out[i * 128:(i + 1) * 128, :], in_=hb[:]), reads=[hbk])

            def tr(e, hb=hb):
                ins = None
                for kc in range(KC):
                    ins = e.transpose(out=ps_tr[:, kc * 128:(kc + 1) * 128], in_=hb[:, kc * 128:(kc + 1) * 128],
                                      identity=ident[:])
                return ins
            S.compute("pe", tr, reads=[hbk, "ident"], writes=["ps_tr"])
            ht = h2T[i % 2]
            htk = f"h2T{i % 2}"
            S.compute("act", lambda e, ht=ht: e.copy(out=ht[:, :, :], in_=ps_tr[:, :].rearrange("p (k t) -> p k t", k=KC)),
                      reads=["ps_tr"], writes=[htk])

            def mmr(e, ht=ht):
                ins = None
                for kc in range(KC):
                    ins = e.matmul(psB[0][:, 0:16], lhsT=ht[:, kc, :], rhs=wr[:, kc, :],
                                   start=(kc == 0), stop=(kc == KC - 1))
                return ins
            S.compute("pe", mmr, reads=[htk, "wr"], writes=["psB0"])
            lgi, exi = lg[i % 2], ex[i % 2]
            lk, ek = f"lg{i % 2}", f"ex{i % 2}"
            mxc, nmc, sec, rsc = (sm[:, 4 * i + k:4 * i + k + 1] for k in range(4))
            S.compute("dve", lambda e, lgi=lgi: e.tensor_tensor(out=lgi[:, :], in0=psB[0][:, 0:16], in1=br_bc[:, :],
                                                                op=ALU.add),
                      reads=["psB0", "br_bc"], writes=[lk])
            S.compute("dve", lambda e, lgi=lgi, mxc=mxc: e.reduce_max(out=mxc, in_=lgi[:, :], axis=AX.X),
                      reads=[lk], writes=[f"sm{i}a"])
            S.compute("dve", lambda e, mxc=mxc, nmc=nmc: e.tensor_scalar(out=nmc, in0=mxc, scalar1=-1.0, scalar2=None,
                                                                         op0=ALU.mult),
                      reads=[f"sm{i}a"], writes=[f"sm{i}b"])
            S.compute("act", lambda e, lgi=lgi, exi=exi, nmc=nmc, sec=sec: e.activation(
                out=exi[:, :], in_=lgi[:, :], func=AF.Exp, bias=nmc, scale=1.0, accum_out=sec),
                reads=[lk, f"sm{i}b"], writes=[ek, f"sm{i}c"])
            S.compute("dve", lambda e, sec=sec, rsc=rsc: e.reciprocal(out=rsc, in_=sec),
                      reads=[f"sm{i}c"], writes=[f"sm{i}d"])
            S.compute("dve", lambda e, exi=exi, rsc=rsc: e.tensor_scalar(out=exi[:, :], in0=exi[:, :], scalar1=rsc,
                                                                         scalar2=None, op0=ALU.mult),
                      reads=[ek, f"sm{i}d"], writes=[ek])
            S.compute("pe", lambda e, exi=exi: e.transpose(out=psB[1][0:16, 0:128], in_=exi[:, :], identity=identf[:]),
                      reads=[ek, "identf"], writes=["psB1"])
            pr = prT[i % 2]
            S.compute("act", lambda e, pr=pr: e.copy(out=pr[:, :], in_=psB[1][0:16, 0:128]),
                      reads=["psB1"], writes=[f"prT{i % 2}"])
            S.dma("sp", lambda e, i=i, pr=pr: e.dma_start(out=probsT_out[:, i * 128:(i + 1) * 128], in_=pr[:, :]),
                  reads=[f"prT{i % 2}"])
        S.emit()
    return nc


NBIS = 28
CAP = 1024


def l4_consts():
    p = np.arange(128)
    c = {}
    c["ones128"] = np.ones((128, 128), np.float32)
    c["striu"] = (p[:, None] < p[None, :]).astype(np.float32)
    c["iota1024"] = np.tile(np.arange(1024, dtype=np.float32)[None, :], (128, 1))
    c["iotaj1"] = (np.arange(8)[None, :] * 128 + p[:, None] + 1).astype(np.float32)
    c["iotaf"] = np.tile(np.arange(64, dtype=np.float32)[None, :], (128, 1))
    c["pidx"] = p[:, None].astype(np.float32)
    c["ident"] = np.eye(128, dtype=NPBF)
    return c


def build_l4():
    nc = bass.Bass("TRN2", target_bir_lowering=False)
    di = lambda name, shape, dt: nc.dram_tensor(name, shape, dt, kind="ExternalInput").ap()
    do = lambda name, shape, dt: nc.dram_tensor(name, shape, dt, kind="ExternalOutput").ap()
    h2_all = di("h2_all", [16384, D], BF16)
    P_in = di("P_in", [128, 4, 64], F32)
    wg_in = di("wg", [2, D, 2048], F32)
    wu_in = di("wu", [2, D, 2048], F32)
    wd_in = di("wd", [2, 2048, D], F32)
    ones_in = di("ones128", [128, 128], F32)
    striu_in = di("striu", [128, 128], F32)
    iota1024_in = di("iota1024", [128, 1024], F32)
    iotaj1_in = di("iotaj1", [128, 8], F32)
    iotaf_in = di("iotaf", [128, 64], F32)
    pidx_in = di("pidx", [128, 1], F32)
    ident_in = di("ident", [128, 128], BF16)
    Y_out = do("Y", [4, CAP, D], F32)
    idx_out = do("idx", [128, 4, 8], I32)
    with ExitStack() as es:
        S = Sched(nc, es)
        sb = lambda name, shape, dt: _sb(nc, es, name, shape, dt)
        ident = sb("ident_sb", [128, 128], BF16)
        ones128 = sb("ones_sb", [128, 128], F32)
        striu = sb("striu_sb", [128, 128], F32)
        iota1024 = sb("iota1024_sb", [128, 1024], F32)
        iotaj1 = sb("iotaj1_sb", [128, 8], F32)
        iotaf = sb("iotaf_sb", [128, 64], F32)
        P4 = sb("P4", [128, 4, 64], F32)
        cmp4 = sb("cmp4", [128, 4, 64], F32)
        cumA = sb("cumA", [128, 4, 64], F32)
        cumB = sb("cumB", [128, 4, 64], F32)
        lo = sb("lo", [128, 4], F32)
        mid = sb("mid", [128, 4], F32)
        cnt = sb("cnt", [128, 4], F32)
        tq = sb("tq", [128, 4], F32)
        off4 = sb("off4", [128, 4], F32)
        offi4 = sb("offi4", [128, 4], F32)
        Rp = sb("Rp", [128, 4, 130], F32)
        oh1 = sb("oh1", [128, 1024], F32)
        ohP = sb("ohP", [128, 1024], F32)
        sel = [sb(f"sel{i}", [128, 64], F32) for i in range(2)]
        sel2 = [sb(f"selb{i}", [128, 64], F32) for i in range(2)]
        smal = sb("smal", [128, 4 * 8 * 4], F32)
        idx_i = sb("idx_i", [128, 4, 8], I32)
        val_sb = sb("val_sb", [128, 4, 8], F32)
        wg = sb("wg_sb", [128, KC, 2048], BF16)
        wu = sb("wu_sb", [128, KC, 2048], BF16)
        wd = sb("wd_sb", [128, 16, D], BF16)
        xe = [sb(f"xe{i}", [128, D], BF16) for i in range(4)]
        xeT = [sb(f"xeT{i}", [128, KC, CAP], BF16) for i in range(2)]
        act = sb("act_sb", [128, 16, 512], BF16)
        sa = [sb(f"sa{i}", [128, 512], F32) for i in range(2)]
        yo = [sb(f"yo{i}", [128, D], F32) for i in range(2)]
        ps_s = _ps(nc, es, "ps_s", [128, 512], F32)
        ps_tr = _ps(nc, es, "ps_tr", [128, D], BF16)
        psG = [_ps(nc, es, f"psG{i}", [128, 512], F32) for i in range(2)]
        psU = [_ps(nc, es, f"psU{i}", [128, 512], F32) for i in range(2)]
        psY = [_ps(nc, es, f"psY{i}", [128, 512], F32) for i in range(2)]

        for t_, src, key in ((ident, ident_in, "ident"), (ones128, ones_in, "ones128"), (striu, striu_in, "striu"),
                             (iota1024, iota1024_in, "iota1024"), (iotaj1, iotaj1_in, "iotaj1"),
                             (iotaf, iotaf_in, "iotaf"), (P4, P_in, "P4")):
            S.dma("sp", lambda e, t_=t_, src=src: e.dma_start(out=t_[:], in_=src), writes=[key])
        for p in range(4):
            S.dma("sp", lambda e, p=p: e.dma_start(out=Rp[:, p, 129:130], in_=pidx_in), writes=[f"Rp{p}"])

        def load_expert(ei):
            for h in range(4):
                S.dma("pool", lambda e, h=h, ei=ei: e.dma_start(
                    out=wg[:, :, h * 512:(h + 1) * 512],
                    in_=wg_in[ei].rearrange("(kc p) n -> p kc n", p=128)[:, :, h * 512:(h + 1) * 512]),
                    writes=["wg"])
                S.dma("pool", lambda e, h=h, ei=ei: e.dma_start(
                    out=wu[:, :, h * 512:(h + 1) * 512],
                    in_=wu_in[ei].rearrange("(kc p) n -> p kc n", p=128)[:, :, h * 512:(h + 1) * 512]),
                    writes=["wu"])
            for h in range(4):
                S.dma("pool", lambda e, h=h, ei=ei: e.dma_start(
                    out=wd[:, h * 4:(h + 1) * 4, :],
                    in_=wd_in[ei].rearrange("(fc p) n -> p fc n", p=128)[:, h * 4:(h + 1) * 4, :]),
                    writes=["wd"])
        load_expert(0)
        S.breg(16383)

        S.compute("dve", lambda e: e.memset(lo[:], 0.0), writes=["lo"])
        for it in range(1, NBIS + 1):
            w = 2.0 ** (-it)
            S.compute("dve", lambda e, w=w: e.tensor_scalar(out=mid[:], in0=lo[:], scalar1=w, scalar2=None, op0=ALU.add),
                      reads=["lo"], writes=["mid"])
            S.compute("dve", lambda e: e.tensor_tensor(out=cmp4[:], in0=P4[:], in1=mid[:, :].unsqueeze(2).broadcast_to([128, 4, 64]),
                                                       op=ALU.is_gt), reads=["P4", "mid"], writes=["cmp4"])
            S.compute("dve", lambda e: e.reduce_sum(out=cnt[:], in_=cmp4[:], axis=AX.X), reads=["cmp4"], writes=["cnt"])
            S.compute("pe", lambda e: e.matmul(ps_s[:, 0:4], lhsT=ones128[:], rhs=cnt[:], start=True, stop=True),
                      reads=["cnt", "ones128"], writes=["ps_s"])
            S.compute("dve", lambda e: e.scalar_tensor_tensor(out=tq[:], in0=ps_s[:, 0:4], scalar=CAP - 0.5, in1=mid[:],
                                                              op0=ALU.is_gt, op1=ALU.mult),
                      reads=["ps_s", "mid"], writes=["tq"])
            S.compute("dve", lambda e: e.tensor_tensor(out=lo[:], in0=lo[:], in1=tq[:], op=ALU.max),
                      reads=["lo", "tq"], writes=["lo"])
        S.compute("dve", lambda e: e.tensor_tensor(out=cmp4[:], in0=P4[:], in1=lo[:, :].unsqueeze(2).broadcast_to([128, 4, 64]),
                                                   op=ALU.is_gt), reads=["P4", "lo"], writes=["cmp4"])
        S.compute("dve", lambda e: e.reduce_sum(out=cnt[:], in_=cmp4[:], axis=AX.X), reads=["cmp4"], writes=["cnt"])
        S.compute("pe", lambda e: e.matmul(ps_s[:, 0:4], lhsT=striu[:], rhs=cnt[:], start=True, stop=True),
                  reads=["cnt", "striu"], writes=["ps_s"])
        S.compute("dve", lambda e: e.tensor_copy(out=off4[:], in_=ps_s[:, 0:4]), reads=["ps_s"], writes=["off4"])
        S.compute("dve", lambda e: e.tensor_tensor(out=offi4[:], in0=off4[:], in1=cnt[:], op=ALU.add),
                  reads=["off4", "cnt"], writes=["offi4"])
        src_t, src_k = cmp4, "cmp4"
        bufs = [(cumA, "cumA"), (cumB, "cumB")]
        for k in range(6):
            s_ = 1 << k
            dst_t, dst_k = bufs[k % 2]
            S.compute("dve", lambda e, s_=s_, src_t=src_t, dst_t=dst_t: e.tensor_copy(out=dst_t[:, :, 0:s_], in_=src_t[:, :, 0:s_]),
                      reads=[src_k], writes=[dst_k])
            S.compute("dve", lambda e, s_=s_, src_t=src_t, dst_t=dst_t: e.tensor_tensor(
                out=dst_t[:, :, s_:64], in0=src_t[:, :, s_:64], in1=src_t[:, :, 0:64 - s_], op=ALU.add),
                reads=[src_k], writes=[dst_k])
            src_t, src_k = dst_t, dst_k
        for p in range(4):
            S.compute("dve", lambda e, p=p, src_t=src_t: e.tensor_tensor(out=Rp[:, p, 0:64], in0=src_t[:, p, :], in1=cmp4[:, p, :],
                                                                         op=ALU.mult),
                      reads=[src_k, "cmp4"], writes=[f"Rp{p}"])
            S.compute("dve", lambda e, p=p: e.tensor_copy(out=Rp[:, p, 64:128], in_=P4[:, p, :]), reads=["P4"], writes=[f"Rp{p}"])
            S.compute("dve", lambda e, p=p: e.tensor_copy(out=Rp[:, p, 128:129], in_=off4[:, p:p + 1]), reads=["off4"], writes=[f"Rp{p}"])
        ectr = [0]
        for p in range(4):
            ei, b = p // 2, p % 2
            if p == 2:
                load_expert(1)
            S.compute("dve", lambda e, p=p: e.tensor_scalar(out=oh1[:], in0=iota1024[:], scalar1=off4[:, p:p + 1], scalar2=None,
                                                            op0=ALU.is_ge), reads=["iota1024", "off4"], writes=["oh1"])
            S.compute("dve", lambda e, p=p: e.scalar_tensor_tensor(out=ohP[:], in0=iota1024[:], scalar=offi4[:, p:p + 1], in1=oh1[:],
                                                                   op0=ALU.is_lt, op1=ALU.mult),
                      reads=["iota1024", "offi4", "oh1"], writes=["ohP"])
            xt_ = xeT[p % 2]
            xtk = f"xeT{p % 2}"
            for jc in range(8):
                S.compute("pe", lambda e, p=p, jc=jc: e.matmul(ps_s[:, 0:130], lhsT=ohP[:, jc * 128:(jc + 1) * 128], rhs=Rp[:, p, :],
                                                               start=True, stop=True),
                          reads=["ohP", f"Rp{p}"], writes=["ps_s"])
                base = (p * 8 + jc) * 4
                r1, fs, vl, ixf = (smal[:, base + k:base + k + 1] for k in range(4))
                sk = f"smal{p}_{jc}"
                s1_, s2_ = sel[jc % 2], sel2[jc % 2]
                S.compute("dve", lambda e, jc=jc, r1=r1: e.tensor_tensor(out=r1, in0=iotaj1[:, jc:jc + 1], in1=ps_s[:, 128:129],
                                                                         op=ALU.subtract),
                          reads=["iotaj1", "ps_s"], writes=[sk + "r"])
                S.compute("dve", lambda e, r1=r1, s1_=s1_: e.tensor_scalar(out=s1_[:], in0=ps_s[:, 0:64], scalar1=r1, scalar2=None,
                                                                           op0=ALU.is_equal),
                          reads=["ps_s", sk + "r"], writes=[f"sel{jc % 2}"])
                S.compute("dve", lambda e, s1_=s1_, s2_=s2_: e.tensor_tensor(out=s2_[:], in0=s1_[:], in1=iotaf[:], op=ALU.mult),
                          reads=[f"sel{jc % 2}", "iotaf"], writes=[f"selb{jc % 2}"])
                S.compute("dve", lambda e, s2_=s2_, fs=fs: e.reduce_sum(out=fs, in_=s2_[:], axis=AX.X),
                          reads=[f"selb{jc % 2}"], writes=[sk + "f"])
                S.compute("dve", lambda e, s1_=s1_, s2_=s2_: e.tensor_tensor(out=s2_[:], in0=s1_[:], in1=ps_s[:, 64:128], op=ALU.mult),
                          reads=[f"sel{jc % 2}", "ps_s"], writes=[f"selb{jc % 2}"])
                S.compute("dve", lambda e, s2_=s2_, p=p, jc=jc: e.reduce_sum(out=val_sb[:, p, jc:jc + 1], in_=s2_[:], axis=AX.X),
                          reads=[f"selb{jc % 2}"], writes=[f"val{p}_{jc}"])
                S.compute("dve", lambda e, fs=fs, ixf=ixf: e.scalar_tensor_tensor(out=ixf, in0=ps_s[:, 129:130], scalar=64.0, in1=fs,
                                                                                  op0=ALU.mult, op1=ALU.add),
                          reads=["ps_s", sk + "f"], writes=[sk + "i"])
                S.compute("dve", lambda e, ixf=ixf, b=b: e.tensor_scalar(out=ixf, in0=ixf, scalar1=float(b * 8192), scalar2=None,
                                                                         op0=ALU.add),
                          reads=[sk + "i"], writes=[sk + "i"])
                S.compute("dve", lambda e, ixf=ixf, p=p, jc=jc: e.tensor_copy(out=idx_i[:, p, jc:jc + 1], in_=ixf),
                          reads=[sk + "i"], writes=[f"idx{p}_{jc}"])
                xg = xe[jc % 4]
                xgk = f"xe{jc % 4}"
                S.dma("pool", lambda e, xg=xg, p=p, jc=jc: e.indirect_dma_start(
                    out=xg[:, :], out_offset=None, in_=h2_all[:, :],
                    in_offset=bass.IndirectOffsetOnAxis(ap=idx_i[:, p, jc:jc + 1], axis=0),
                    bounds_check=S.pregs[16383], oob_is_err=False),
                    reads=[f"idx{p}_{jc}"], writes=[xgk])

                def tr(e, xg=xg):
                    ins = None
                    for kc in range(KC):
                        ins = e.transpose(out=ps_tr[:, kc * 128:(kc + 1) * 128], in_=xg[:, kc * 128:(kc + 1) * 128],
                                          identity=ident[:])
                    return ins
                S.compute("pe", tr, reads=[xgk, "ident"], writes=["ps_tr"])
                S.compute("act", lambda e, xt_=xt_, jc=jc: e.copy(out=xt_[:, :, jc * 128:(jc + 1) * 128],
                                                                  in_=ps_tr[:, :].rearrange("p (k t) -> p k t", k=KC)),
                          reads=["ps_tr"], writes=[xtk + f"_{jc}"])
            allx = [xtk + f"_{jc}" for jc in range(8)]
            for tb in range(2):
                c0 = tb * 512
                for fc in range(16):
                    pg, pu = psG[ectr[0] % 2], psU[ectr[0] % 2]
                    pgk, puk = f"psG{ectr[0] % 2}", f"psU{ectr[0] % 2}"
                    sa_ = sa[ectr[0] % 2]
                    sak = f"sa{ectr[0] % 2}"
                    ectr[0] += 1

                    def mmg(e, pg=pg, fc=fc, c0=c0, xt_=xt_, w_=wg):
                        ins = None
                        for kc in range(KC):
                            ins = e.matmul(pg[:, :], lhsT=w_[:, kc, fc * 128:(fc + 1) * 128], rhs=xt_[:, kc, c0:c0 + 512],
                                           start=(kc == 0), stop=(kc == KC - 1))
                        return ins
                    S.compute("pe", mmg, reads=allx + ["wg"], writes=[pgk])
                    S.compute("pe", lambda e, pu=pu, fc=fc, c0=c0, xt_=xt_: mmg(e, pu, fc, c0, xt_, wu),
                              reads=allx + ["wu"], writes=[puk])
                    S.compute("act", lambda e, sa_=sa_, pg=pg: e.activation(out=sa_[:, :], in_=pg[:, :], func=AF.Silu),
                              reads=[pgk], writes=[sak])
                    S.compute("dve", lambda e, sa_=sa_, pu=pu, fc=fc: e.tensor_tensor(out=act[:, fc, :], in0=sa_[:, :], in1=pu[:, :],
                                                                                      op=ALU.mult),
                              reads=[sak, puk], writes=[f"act{fc}"])
                allact = [f"act{fc}" for fc in range(16)]
                for tt in range(4):
                    jc = tb * 4 + tt
                    yo_ = yo[jc % 2]
                    yok = f"yo{jc % 2}"
                    for dh in range(2):
                        py = psY[dh]

                        def mmd(e, py=py, tt=tt, dh=dh):
                            ins = None
                            for fc in range(16):
                                ins = e.matmul(py[:, :], lhsT=act[:, fc, tt * 128:(tt + 1) * 128],
                                               rhs=wd[:, fc, dh * 512:(dh + 1) * 512], start=(fc == 0), stop=(fc == 15))
                            return ins
                        S.compute("pe", mmd, reads=allact + ["wd"], writes=[f"psY{dh}"])
                        S.compute("act", lambda e, yo_=yo_, py=py, dh=dh, p=p, jc=jc: e.activation(
                            out=yo_[:, dh * 512:(dh + 1) * 512], in_=py[:, :], func=AF.Copy, scale=val_sb[:, p, jc:jc + 1]),
                            reads=[f"psY{dh}", f"val{p}_{jc}"], writes=[yok])
                    S.dma("sp", lambda e, yo_=yo_, p=p, jc=jc: e.dma_start(out=Y_out[p, jc * 128:(jc + 1) * 128, :], in_=yo_[:, :]),
                          reads=[yok])
        S.dma("sp", lambda e: e.dma_start(out=idx_out, in_=idx_i[:]), reads=[f"idx{p}_{jc}" for p in range(4) for jc in range(8)])
        S.emit()
    return nc


def build_l5():
    nc = bass.Bass("TRN2", target_bir_lowering=False)
    di = lambda name, shape, dt: nc.dram_tensor(name, shape, dt, kind="ExternalInput").ap()
    xmid = di("x_mid", [NT, D], F32)
    Yb = di("Yb", [16 * CAP, D], F32)
    idx_in = di("idxb", [128, 128], I32)
    base_in = di("base", [128, 1], F32)
    mod_in = di("mod", [1, 6144], F32)
    gfin_in = di("g_final", [1, D], F32)
    out = nc.dram_tensor("out", [NT, D], F32, kind="ExternalOutput").ap()
    acc = nc.dram_tensor("acc", [NT, D], F32).ap()
    with ExitStack() as es:
        S = Sched(nc, es)
        sb = lambda name, shape, dt: _sb(nc, es, name, shape, dt)
        zt = sb("zt", [128, D], F32)
        idx_i = sb("idx_i", [128, 128], I32)
        idx_f = sb("idx_f", [128, 128], F32)
        neg_f = sb("neg_f", [128, 128], F32)
        loci = sb("loci", [128, 128], I32)
        base = sb("base_sb", [128, 1], F32)
        gf_bc = sb("gf_bc", [128, D], F32)
        gfin_bc = sb("gfin_bc", [128, D], F32)
        yt = [sb(f"yt{i}", [128, D], F32) for i in range(4)]
        at = [sb(f"at{i}", [128, D], F32) for i in range(2)]
        xt = [sb(f"xt{i}", [128, D], F32) for i in range(2)]
        junk = sb("junk", [128, D], BF16)
        ss = sb("ss", [128, NTILE], F32)
        rstd = sb("rstd", [128, NTILE], F32)
        S.breg(NT - 1)
        S.compute("dve", lambda e: e.memset(zt[:], 0.0), writes=["zt"])
        for i in range(NTILE):
            S.dma("sp", lambda e, i=i: e.dma_start(out=acc[i * 128:(i + 1) * 128, :], in_=zt[:]), reads=["zt"], writes=["acc"])
        S.dma("sp", lambda e: e.dma_start(out=idx_i[:], in_=idx_in), writes=["idx_i"])
        S.dma("sp", lambda e: e.dma_start(out=base[:], in_=base_in), writes=["base"])
        S.dma("sp", lambda e: e.dma_start(out=gf_bc[:], in_=mod_in[0:1, 5120:6144].partition_broadcast(128)), writes=["gf_bc"])
        S.dma("sp", lambda e: e.dma_start(out=gfin_bc[:], in_=gfin_in[0:1, :].partition_broadcast(128)), writes=["gfin_bc"])
        S.compute("dve", lambda e: e.tensor_copy(out=idx_f[:], in_=idx_i[:]), reads=["idx_i"], writes=["idx_f"])
        S.compute("dve", lambda e: e.tensor_scalar(out=idx_f[:], in0=idx_f[:], scalar1=base[:, 0:1], scalar2=None, op0=ALU.subtract),
                  reads=["idx_f", "base"], writes=["idx_f"])
        S.compute("dve", lambda e: e.tensor_scalar(out=neg_f[:], in0=idx_f[:], scalar1=0.0, scalar2=1.0e6, op0=ALU.is_lt, op1=ALU.mult),
                  reads=["idx_f"], writes=["neg_f"])
        S.compute("dve", lambda e: e.tensor_tensor(out=idx_f[:], in0=idx_f[:], in1=neg_f[:], op=ALU.add),
                  reads=["idx_f", "neg_f"], writes=["idx_f"])
        S.compute("dve", lambda e: e.tensor_copy(out=loci[:], in_=idx_f[:]), reads=["idx_f"], writes=["loci"])
        for k in range(128):
            yb = yt[k % 4]
            ybk = f"yt{k % 4}"
            S.dma("sp", lambda e, k=k, yb=yb: e.dma_start(out=yb[:], in_=Yb[k * 128:(k + 1) * 128, :]), writes=[ybk])
            S.dma("pool", lambda e, k=k, yb=yb: e.indirect_dma_start(
                out=acc[:, :], out_offset=bass.IndirectOffsetOnAxis(ap=loci[:, k:k + 1], axis=0),
                in_=yb[:, :], in_offset=None, bounds_check=S.pregs[NT - 1], oob_is_err=False, compute_op=ALU.add),
                reads=[ybk, "loci"], writes=["acc"])
        for i in range(NTILE):
            a_, x_ = at[i % 2], xt[i % 2]
            ak, xk = f"at{i % 2}", f"xt{i % 2}"
            S.dma("sp", lambda e, i=i, a_=a_: e.dma_start(out=a_[:], in_=acc[i * 128:(i + 1) * 128, :]), reads=["acc"], writes=[ak])
            S.dma("sp", lambda e, i=i, x_=x_: e.dma_start(out=x_[:], in_=xmid[i * 128:(i + 1) * 128, :]), writes=[xk])
            S.compute("dve", lambda e, a_=a_: e.tensor_tensor(out=a_[:], in0=a_[:], in1=gf_bc[:], op=ALU.mult),
                      reads=[ak, "gf_bc"], writes=[ak])
            S.compute("pool", lambda e, a_=a_, x_=x_: e.tensor_tensor(out=x_[:], in0=a_[:], in1=x_[:], op=ALU.add),
                      reads=[ak, xk], writes=[xk])
            S.compute("act", lambda e, x_=x_, i=i: e.activation(out=junk[:], in_=x_[:], func=AF.Square, accum_out=ss[:, i:i + 1]),
                      reads=[xk], writes=["junk", f"ss{i}"])
            S.compute("act", lambda e, i=i: e.activation(out=rstd[:, i:i + 1], in_=ss[:, i:i + 1], func=AF.Sqrt, scale=1.0 / D, bias=EPS),
                      reads=[f"ss{i}"], writes=[f"rstd{i}"])
            S.compute("dve", lambda e, i=i: e.reciprocal(out=rstd[:, i:i + 1], in_=rstd[:, i:i + 1]), reads=[f"rstd{i}"], writes=[f"rstd{i}"])
            S.compute("dve", lambda e, x_=x_, a_=a_, i=i: e.scalar_tensor_tensor(out=a_[:], in0=x_[:], scalar=rstd[:, i:i + 1], in1=gfin_bc[:],
                                                                                 op0=ALU.mult, op1=ALU.mult),
                      reads=[xk, f"rstd{i}", "gfin_bc"], writes=[ak])
            S.dma("sp", lambda e, i=i, a_=a_: e.dma_start(out=out[i * 128:(i + 1) * 128, :], in_=a_[:]), reads=[ak])
        S.emit()
    return nc


_PROGS = {}


def _prog(name, fn):
    if name not in _PROGS:
        _PROGS[name] = fn()
    return _PROGS[name]


def kernel(x, c, w_ada, b_ada, g_norm_mix, w_in, b_gate, w_fourier, w_conv, w_conv_out, w_o,
           g_norm_moe, w_router, b_router, w_gate_e, w_up_e, w_down_e, g_final):
    f32 = np.float32
    x = np.asarray(x, f32); c = np.asarray(c, f32)
    w_ada = np.asarray(w_ada, f32)[0]; b_ada = np.asarray(b_ada, f32)[0]
    g_mix = np.asarray(g_norm_mix, f32)[0]; w_in = np.asarray(w_in, f32)[0]
    b_gate = np.asarray(b_gate, f32)[0]; w_fourier = np.asarray(w_fourier, f32)[0]
    w_conv = np.asarray(w_conv, f32)[0]; w_conv_out = np.asarray(w_conv_out, f32)[0]
    w_o = np.asarray(w_o, f32)[0]; g_moe = np.asarray(g_norm_moe, f32)[0]
    w_router = np.asarray(w_router, f32)[0]; b_router = np.asarray(b_router, f32)[0]
    w_gate_e = np.asarray(w_gate_e, f32)[0]; w_up_e = np.asarray(w_up_e, f32)[0]; w_down_e = np.asarray(w_down_e, f32)[0]
    g_final = np.asarray(g_final, f32)
    cores = list(range(8))
    ident = np.eye(128, dtype=NPBF)
    identf = np.eye(128, dtype=f32)
    in1 = []
    for r in cores:
        b, j = r // 4, r % 4
        in1.append({"x": np.ascontiguousarray(x[b, j * NT:(j + 1) * NT]),
                    "cT": np.ascontiguousarray(c[b].reshape(8, 128).T),
                    "w_ada": w_ada, "b_ada": b_ada[None, :], "g_mix": g_mix[None, :],
                    "w_inf": np.ascontiguousarray(w_in[:, :512]), "ident": ident})
    r1 = run_bass_kernel_spmd(_prog("l1", build_l1), in1, core_ids=cores).results
    u_f = np.stack([np.concatenate([np.asarray(r1[4 * b + j]["u_f"]) for j in range(4)], 0) for b in range(2)])
    mods = [np.asarray(r1[4 * b]["mod_out"]) for b in range(2)]
    f1, stab, cs = dft_tables()
    in2 = []
    for r in cores:
        b, g = r // 4, r % 4
        in2.append({"U": np.ascontiguousarray(u_f[b, :, g * 128:(g + 1) * 128]), "f1": f1, "stab": stab, "cs": cs})
    r2 = run_bass_kernel_spmd(_prog("l2", build_l2), in2, core_ids=cores).results
    in3 = []
    for r in cores:
        b, j = r // 4, r % 4
        xh = np.zeros((128, D), f32)
        hm = np.zeros((128, 2), f32)
        if j > 0:
            xh[0] = x[b, j * NT - 1]; hm[:, 0] = 1.0
        if j < 3:
            xh[1] = x[b, (j + 1) * NT]; hm[:, 1] = 1.0
        AT = np.ascontiguousarray(np.stack([np.asarray(r2[4 * b + g]["A_out"])[:, j * NT:(j + 1) * NT] for g in range(4)], 1))
        in3.append({"x": in1[r]["x"], "xh": xh, "hmask": hm, "mod": mods[b], "g_mix": g_mix[None, :], "g_moe": g_moe[None, :],
                    "w_in": w_in, "bgate": np.ascontiguousarray(b_gate.reshape(16, 128).T),
                    "wconv": np.ascontiguousarray(w_conv.reshape(3, 8, 128).transpose(2, 1, 0)),
                    "w_co": w_conv_out, "w_fo": w_fourier, "w_o": w_o, "w_r": w_router, "b_r": b_router[None, :],
                    "AT": AT, "ident": ident, "identf": identf})
    r3 = run_bass_kernel_spmd(_prog("l3", build_l3), in3, core_ids=cores).results
    h2_all = np.concatenate([np.asarray(r3[r]["h2"]) for r in cores], 0)
    probsT = [np.concatenate([np.asarray(r3[4 * b + j]["probsT"]) for j in range(4)], 1) for b in range(2)]
    cons = l4_consts()
    in4 = []
    for q in cores:
        P_in = np.zeros((128, 4, 64), f32)
        for p in range(4):
            ei, b = p // 2, p % 2
            P_in[:, p, :] = probsT[b][2 * q + ei].reshape(128, 64)
        mp = {"h2_all": h2_all, "P_in": P_in, "wg": w_gate_e[2 * q:2 * q + 2], "wu": w_up_e[2 * q:2 * q + 2],
              "wd": w_down_e[2 * q:2 * q + 2]}
        mp.update(cons)
        in4.append(mp)
    r4 = run_bass_kernel_spmd(_prog("l4", build_l4), in4, core_ids=cores).results
    in5 = []
    for r in cores:
        b, j = r // 4, r % 4
        Yb = np.concatenate([np.asarray(r4[e // 2]["Y"])[(e % 2) * 2 + b] for e in range(16)], 0)
        idxb = np.stack([np.asarray(r4[e // 2]["idx"])[:, (e % 2) * 2 + b, :] for e in range(16)], 1)
        in5.append({"x_mid": np.asarray(r3[r]["x_mid"]), "Yb": Yb, "idxb": np.ascontiguousarray(idxb.reshape(128, 128)),
                    "base": np.full((128, 1), b * 8192 + j * NT, f32), "mod": mods[b], "g_final": g_final[None, :]})
    r5 = run_bass_kernel_spmd(_prog("l5", build_l5), in5, core_ids=cores).results
    out = np.stack([np.concatenate([np.asarray(r5[4 * b + j]["out"]) for j in range(4)], 0) for b in range(2)])
    return out.astype(f32)
```
